# Optimizing a Trainium2 kernel written in Bass

```python
import math
import jax, jax.numpy as jnp
from jax import lax
import numpy as np

D_MODEL = 4096
BATCH = 2
SEQ = 4096
DEPTH = 2

HEAD_DIM = 128
A_GROUPS = ((128, 1), (512, 4), (2048, 16))
A_HEADS = D_MODEL // 256
B_HEADS = D_MODEL // HEAD_DIM
D_FF = ((8 * D_MODEL // 3 + 255) // 256) * 256
N_EXPERTS = 8
TOP_K = 2
D_EXPERT = 3 * D_MODEL // 2
PLE_DIM = 256
ROPE_THETA = 10000.0
EPS = 1e-6
BLOCK = 128
N_A = DEPTH // 2
N_B = DEPTH - N_A
N_DENSE = (DEPTH + 1) // 2
N_MOE = DEPTH // 2

kernel_name = 'yoco_dilated_stickbreaking_moe_block'


def rms_norm(x, g):
    xf = x.astype(jnp.float32)
    y = xf * lax.rsqrt(jnp.mean(xf * xf, axis=-1, keepdims=True) + EPS)
    return (y * g.astype(jnp.float32)).astype(x.dtype)


def rope(x, pos):
    half = HEAD_DIM // 2
    inv = ROPE_THETA ** (-jnp.arange(half, dtype=jnp.float32) / half)
    ang = pos.astype(jnp.float32)[:, None] * inv[None, :]
    cos = jnp.cos(ang)[None, :, None, None, :]
    sin = jnp.sin(ang)[None, :, None, None, :]
    xf = x.astype(jnp.float32)
    x1, x2 = xf[..., :half], xf[..., half:]
    return jnp.concatenate([x1 * cos - x2 * sin, x2 * cos + x1 * sin], axis=-1)


def dilated_window_attention(q, k, v, span, dil):
    Bsz, S, H, D = q.shape
    L = S // dil
    nb = -(-L // BLOCK)
    Lp = nb * BLOCK
    w = span // dil

    def to_sub(t):
        return t.reshape(Bsz, L, dil, H, D).transpose(0, 2, 1, 3, 4)

    qs = jnp.pad(to_sub(q), ((0, 0), (0, 0), (0, Lp - L), (0, 0), (0, 0)))
    qs = qs.reshape(Bsz, dil, nb, BLOCK, H, D)

    def key_blocks(t):
        ts = jnp.pad(to_sub(t), ((0, 0), (0, 0), (BLOCK, Lp - L), (0, 0), (0, 0)))
        ts = ts.reshape(Bsz, dil, nb + 1, BLOCK, H, D)
        return jnp.concatenate([ts[:, :, :-1], ts[:, :, 1:]], axis=3)

    ks, vs = key_blocks(k), key_blocks(v)
    s = jnp.einsum('bdnqhc,bdnkhc->bdnhqk', qs, ks) / math.sqrt(D)
    i = jnp.arange(BLOCK)[:, None]
    j = jnp.arange(2 * BLOCK)[None, :]
    dist = BLOCK + i - j
    key_idx = jnp.arange(nb)[:, None, None] * BLOCK + j[None] - BLOCK
    mask = (dist >= 0) & (dist <= w) & (key_idx >= 0)
    s = jnp.where(mask[:, None], s, -jnp.inf)
    m = jnp.max(s, axis=-1, keepdims=True)
    pexp = jnp.exp(s - m)
    l = jnp.sum(pexp, axis=-1)
    o = jnp.einsum('bdnhqk,bdnkhc->bdnqhc', pexp, vs)
    o = o / jnp.transpose(l, (0, 1, 2, 4, 3))[..., None]
    lse = jnp.transpose(m[..., 0] + jnp.log(l), (0, 1, 2, 4, 3))
    o = o.reshape(Bsz, dil, Lp, H, D)[:, :, :L].transpose(0, 2, 1, 3, 4).reshape(Bsz, S, H, D)
    lse = lse.reshape(Bsz, dil, Lp, H)[:, :, :L].transpose(0, 2, 1, 3).reshape(Bsz, S, H)
    return o, lse


def dilated_mixer(hn, w_qkv, g_q, g_k, w_o, pos):
    Bsz, S, _ = hn.shape
    G = len(A_GROUPS)
    qkv = (hn @ w_qkv).reshape(Bsz, S, 3, G, A_HEADS, HEAD_DIM)
    q = rope(rms_norm(qkv[:, :, 0], g_q), pos)
    k = rope(rms_norm(qkv[:, :, 1], g_k), pos)
    v = qkv[:, :, 2].astype(jnp.float32)
    outs, lses = [], []
    for g, (span, dil) in enumerate(A_GROUPS):
        o_g, l_g = dilated_window_attention(q[:, :, g], k[:, :, g], v[:, :, g], span, dil)
        outs.append(o_g)
        lses.append(l_g)
    o = jnp.stack(outs, axis=2)
    wgt = jax.nn.softmax(jnp.stack(lses, axis=2), axis=2)
    o = jnp.einsum('bsgh,bsghd->bshd', wgt, o).reshape(Bsz, S, A_HEADS * HEAD_DIM)
    return o.astype(hn.dtype) @ w_o


def stick_breaking(q, k, v):
    Bsz, S, H, D = q.shape
    nq = S // BLOCK
    qf = q.astype(jnp.float32).reshape(Bsz, nq, BLOCK, H, D).transpose(1, 0, 3, 2, 4)
    kf = k.astype(jnp.float32).transpose(0, 2, 1, 3)
    vf = v.astype(jnp.float32).transpose(0, 2, 1, 3)
    key_pos = jnp.arange(S)
    scale = 1.0 / math.sqrt(D)

    def block(args):
        qb, n = args
        z = jnp.einsum('bhqd,bhkd->bhqk', qb, kf) * scale
        qpos = n * BLOCK + jnp.arange(BLOCK)
        strict = key_pos[None, :] < qpos[:, None]
        log_keep = jnp.where(strict, jax.nn.log_sigmoid(-z), 0.0)
        later = lax.cumsum(log_keep, axis=3, reverse=True) - log_keep
        a = jnp.where(strict, jnp.exp(jax.nn.log_sigmoid(z) + later), 0.0)
        return jnp.einsum('bhqk,bhkd->bhqd', a, vf)

    o = lax.map(block, (qf, jnp.arange(nq)))
    return o.transpose(1, 0, 3, 2, 4).reshape(Bsz, S, H * D).astype(q.dtype)


def swiglu(x, w_gate, w_up, w_down):
    return (jax.nn.silu(x @ w_gate) * (x @ w_up)) @ w_down


def moe_swiglu(hn, w_router, w_gate, w_up, w_down):
    logits = hn.astype(jnp.float32) @ w_router.astype(jnp.float32)
    top_v, top_i = lax.top_k(logits, TOP_K)
    probs = jax.nn.softmax(top_v, axis=-1)
    gates = jnp.sum(jax.nn.one_hot(top_i, N_EXPERTS, dtype=jnp.float32) * probs[..., None], axis=-2)
    y = jnp.zeros_like(hn)
    for e in range(N_EXPERTS):
        y = y + gates[..., e:e + 1].astype(hn.dtype) * swiglu(hn, w_gate[e], w_up[e], w_down[e])
    return y


def per_layer_embedding(h, p_i, g_norm, w_up, w_gdown, w_gup):
    gate = jax.nn.sigmoid((rms_norm(h, g_norm) @ w_gdown) @ w_gup)
    return h + gate * (p_i @ w_up)


def setup_inputs(seed: int = 0) -> dict:
    key = jax.random.key(seed)
    ks = jax.random.split(key, 24)

    def w(k, shape, fan_in):
        return jax.random.normal(k, shape, jnp.float32) * (fan_in ** -0.5)

    def gain(k, shape):
        return 1.0 + 0.02 * jax.random.normal(k, shape, jnp.float32)

    a_qkv = 3 * len(A_GROUPS) * A_HEADS * HEAD_DIM
    a_out = A_HEADS * HEAD_DIM
    b_w = B_HEADS * HEAD_DIM
    return {
        'x': jax.random.normal(ks[0], (BATCH, SEQ, D_MODEL), jnp.float32),
        'p': jax.random.normal(ks[1], (DEPTH, BATCH, SEQ, PLE_DIM), jnp.float32),
        'norm_mix': gain(ks[2], (DEPTH, D_MODEL)),
        'norm_ffn': gain(ks[3], (DEPTH, D_MODEL)),
        'norm_ple': gain(ks[4], (DEPTH, D_MODEL)),
        'a_w_qkv': w(ks[5], (N_A, D_MODEL, a_qkv), D_MODEL),
        'a_q_norm': gain(ks[6], (N_A, HEAD_DIM)),
        'a_k_norm': gain(ks[7], (N_A, HEAD_DIM)),
        'a_w_o': w(ks[8], (N_A, a_out, D_MODEL), a_out),
        'kv_norm': gain(ks[9], (D_MODEL,)),
        'w_kv': w(ks[10], (D_MODEL, 2 * b_w), D_MODEL),
        'b_w_q': w(ks[11], (N_B, D_MODEL, b_w), D_MODEL),
        'b_w_o': w(ks[12], (N_B, b_w, D_MODEL), b_w),
        'ffn_w_gate': w(ks[13], (N_DENSE, D_MODEL, D_FF), D_MODEL),
        'ffn_w_up': w(ks[14], (N_DENSE, D_MODEL, D_FF), D_MODEL),
        'ffn_w_down': w(ks[15], (N_DENSE, D_FF, D_MODEL), D_FF),
        'moe_w_router': w(ks[16], (N_MOE, D_MODEL, N_EXPERTS), D_MODEL),
        'moe_w_gate': w(ks[17], (N_MOE, N_EXPERTS, D_MODEL, D_EXPERT), D_MODEL),
        'moe_w_up': w(ks[18], (N_MOE, N_EXPERTS, D_MODEL, D_EXPERT), D_MODEL),
        'moe_w_down': w(ks[19], (N_MOE, N_EXPERTS, D_EXPERT, D_MODEL), D_EXPERT),
        'ple_w_up': w(ks[20], (DEPTH, PLE_DIM, D_MODEL), PLE_DIM),
        'ple_w_gdown': w(ks[21], (DEPTH, D_MODEL, PLE_DIM), D_MODEL),
        'ple_w_gup': w(ks[22], (DEPTH, PLE_DIM, D_MODEL), PLE_DIM),
    }


def reference(x, p, norm_mix, norm_ffn, norm_ple, a_w_qkv, a_q_norm, a_k_norm, a_w_o,
              kv_norm, w_kv, b_w_q, b_w_o, ffn_w_gate, ffn_w_up, ffn_w_down,
              moe_w_router, moe_w_gate, moe_w_up, moe_w_down,
              ple_w_up, ple_w_gdown, ple_w_gup):
    Bsz, S, _ = x.shape
    pos = jnp.arange(S)
    h = x
    k_s = v_s = None
    for i in range(DEPTH):
        if i == N_A:
            kv = (rms_norm(h, kv_norm) @ w_kv).reshape(Bsz, S, 2, B_HEADS, HEAD_DIM)
            k_s, v_s = kv[:, :, 0], kv[:, :, 1]
        hn = rms_norm(h, norm_mix[i])
        if i < N_A:
            h = h + dilated_mixer(hn, a_w_qkv[i], a_q_norm[i], a_k_norm[i], a_w_o[i], pos)
        else:
            j = i - N_A
            q = (hn @ b_w_q[j]).reshape(Bsz, S, B_HEADS, HEAD_DIM)
            h = h + stick_breaking(q, k_s, v_s) @ b_w_o[j]
        hn = rms_norm(h, norm_ffn[i])
        if i % 2 == 0:
            c = i // 2
            h = h + swiglu(hn, ffn_w_gate[c], ffn_w_up[c], ffn_w_down[c])
        else:
            c = i // 2
            h = h + moe_swiglu(hn, moe_w_router[c], moe_w_gate[c], moe_w_up[c], moe_w_down[c])
        h = per_layer_embedding(h, p[i], norm_ple[i], ple_w_up[i], ple_w_gdown[i], ple_w_gup[i])
    return h
```

```python
import contextlib
import numpy as np
import concourse.bass as bass
import concourse.mybir as mybir
from concourse.bass_utils import run_bass_kernel_spmd

F32 = mybir.dt.float32
BF = mybir.dt.bfloat16
ALU = mybir.AluOpType
AF = mybir.ActivationFunctionType
NCORE = 8
EPS = 1e-6
FAKE_AR = False


class Buf:
    __slots__ = ("w", "r", "excl")

    def __init__(self, excl=False):
        self.w = None
        self.r = {}
        self.excl = excl


class Eng:
    def __init__(self, P, name, e, self_sync):
        self.name = name
        self.e = e
        self.sem = P.new_sem("s_" + name)
        self.key = "E" + name
        self.count = 0
        self.waited = {}
        self.self_sync = self_sync


class DmaSem:
    def __init__(self, P, name):
        self.sem = P.new_sem(name)
        self.key = name
        self.val = 0


class DmaQ:
    def __init__(self, P, name, eng, k):
        self.eng = eng
        self.ring = [DmaSem(P, "d_%s%d" % (name, i)) for i in range(k)]
        self.i = 0


class Prog:
    def __init__(self):
        self.nc = bass.Bass("TRN2", target_bir_lowering=False)
        self.es = contextlib.ExitStack()
        self.nsem = 0
        nc = self.nc
        self.pe = Eng(self, "pe", nc.tensor, False)
        self.act = Eng(self, "act", nc.scalar, True)
        self.dve = Eng(self, "dve", nc.vector, True)
        self.pool = Eng(self, "pool", nc.gpsimd, True)
        self.sp = Eng(self, "sp", nc.sync, False)
        self.engs = [self.pe, self.act, self.dve, self.pool, self.sp]
        self.qsp = DmaQ(self, "sp", self.sp, 8)
        self.qpool = DmaQ(self, "pl", self.pool, 8)
        self.qact = DmaQ(self, "ac", self.act, 4)
        self.ccq = [DmaSem(self, "cc%d" % i) for i in range(4)]
        self.cci = 0

    def new_sem(self, name):
        self.nsem += 1
        return self.es.enter_context(self.nc.semaphore(name))

    def sb(self, es, name, shape, dt):
        return es.enter_context(self.nc.sbuf_tensor(name, shape, dt))

    def _wait(self, eng, tok, war=False):
        if tok is None:
            return
        sem, val, key = tok
        if key == eng.key and (war or not eng.self_sync):
            return
        if eng.waited.get(key, 0) >= val:
            return
        eng.e.wait_ge(sem, val)
        eng.waited[key] = val

    def _deps(self, eng, reads, writes):
        for b in reads:
            self._wait(eng, b.w)
        for b in writes:
            self._wait(eng, b.w)
            for t in b.r.values():
                self._wait(eng, t, war=True)

    def _mark(self, tok, reads, writes):
        key = tok[2]
        for b in reads:
            b.r[key] = tok
        for b in writes:
            b.w = tok
            b.r = {}

    def op(self, eng, fn, reads=(), writes=(), inc=True):
        ex = [b for b in reads if b.excl]
        if ex:
            reads = [b for b in reads if not b.excl]
            writes = list(writes) + ex
        self._deps(eng, reads, writes)
        ins = fn()
        if inc:
            eng.count += 1
            ins.then_inc(eng.sem, 1)
            val = eng.count
        else:
            val = eng.count + 1
        self._mark((eng.sem, val, eng.key), reads, writes)
        return ins

    def dma(self, q, out, in_, reads=(), writes=()):
        eng = q.eng
        self._deps(eng, reads, writes)
        s = q.ring[q.i % len(q.ring)]
        q.i += 1
        if s.val > 0:
            self._wait(eng, (s.sem, s.val, s.key))
        ins = eng.e.dma_start(out=out, in_=in_)
        s.val += 16
        ins.then_inc(s.sem, 16)
        self._mark((s.sem, s.val, s.key), reads, writes)

    def allreduce(self, in_ap, out_ap, reads=(), writes=()):
        if FAKE_AR:
            return self.dma(self.qpool, out_ap, in_ap, reads=reads, writes=writes)
        eng = self.pool
        self._deps(eng, reads, writes)
        s = self.ccq[self.cci % len(self.ccq)]
        self.cci += 1
        if s.val > 0:
            self._wait(eng, (s.sem, s.val, s.key))
        ins = eng.e.collective_compute("AllReduce", ALU.add, replica_groups=[list(range(NCORE))],
                                       ins=[in_ap.opt()], outs=[out_ap.opt()])
        s.val += 1
        ins.then_inc(s.sem, 1)
        self._mark((s.sem, s.val, s.key), reads, writes)

    def barrier(self):
        toks = [(e.sem, e.count, e.key) for e in self.engs if e.count > 0]
        for q in (self.qsp, self.qpool, self.qact):
            toks += [(s.sem, s.val, s.key) for s in q.ring if s.val > 0]
        toks += [(s.sem, s.val, s.key) for s in self.ccq if s.val > 0]
        for e in self.engs:
            for t in toks:
                if t[2] == e.key and not e.self_sync:
                    continue
                sem, val, key = t
                if e.waited.get(key, 0) >= val:
                    continue
                e.e.wait_ge(sem, val)
                e.waited[key] = val

    def finish(self):
        self.barrier()
        self.es.close()
        return self.nc


class Cfg:
    def __init__(self, D=4096, B=2, S=4096, DFF=11008, DEXP=6144, NEXP=8, PLE=256,
                 groups=((128, 1), (512, 4), (2048, 16))):
        self.D, self.B, self.S = D, B, S
        self.NT = B * S
        self.KC = D // 128
        self.NG = D // 512
        self.TW = 512
        self.NTI = self.NT // 512
        self.TB = self.NT // NCORE
        self.AH = D // 256
        self.AHC = self.AH // NCORE
        self.BH = D // 128
        self.BHC = self.BH // NCORE
        self.groups = groups
        self.NGR = len(groups)
        self.DFF = DFF
        self.FC = -(-(DFF // NCORE) // 128)
        self.FS = DFF // NCORE
        self.DEXP = DEXP
        self.EC = DEXP // 128
        self.NEXP = NEXP
        self.PLE = PLE
        self.PC = PLE // 128


GM = 4


def group_weights(W, KCpad=None):
    K, M = W.shape
    KC = -(-K // 128)
    if KCpad:
        KC = KCpad
    G = -(-M // 512)
    Wp = np.zeros((KC * 128, G * 512), np.float32)
    Wp[:K, :M] = W
    return np.ascontiguousarray(Wp.reshape(KC, 128, G, 512).transpose(2, 1, 0, 3))


class K:
    def __init__(self, cfg):
        self.c = cfg
        self.P = Prog()
        self.nc = self.P.nc
        self.ins = {}
        self.uid = 0

    def din(self, name, shape, dt=F32):
        t = self.nc.dram_tensor(name, list(shape), dt, kind="ExternalInput")
        self.ins[name] = t
        return t

    def dscr(self, name, shape, dt):
        return self.nc.dram_tensor(name, list(shape), dt)

    def name(self, s):
        self.uid += 1
        return "%s_%d" % (s, self.uid)


def gemm(kb, tag, Wd, KC, G, mchunks_of, xsrc, xbufs_of, epi, NTI, SL=16):
    P, nc = kb.P, kb.nc
    with contextlib.ExitStack() as es:
        Wb = [P.sb(es, kb.name(tag + "W"), [128, KC, 512], BF) for _ in range(2)]
        Wbuf = [Buf(), Buf()]
        NR = 4
        Xr = [P.sb(es, kb.name(tag + "X"), [128, SL, 512], BF) for _ in range(NR)]
        Xbuf = [Buf() for _ in range(NR)]
        xi = 0
        slabs = [(k0, min(k0 + SL, KC)) for k0 in range(0, KC, SL)]

        def load_w(g):
            wb = Wb[g % 2]
            for k0 in range(0, KC, 8):
                k1 = min(k0 + 8, KC)
                P.dma(P.qpool, wb[:, k0:k1, :], Wd[g, :, k0:k1, :], writes=[Wbuf[g % 2]])

        load_w(0)
        tcount = 0
        for g in range(G):
            if g + 1 < G:
                load_w(g + 1)
            nm = mchunks_of(g)
            wb = Wb[g % 2]
            for ti in range(NTI):
                base = (tcount % 2) * 4
                tcount += 1
                banks = [kb.bank(base + m) for m in range(nm)]
                bbufs = [kb.bankbuf[base + m] for m in range(nm)]
                for (k0, k1) in slabs:
                    xs = Xr[xi % NR]
                    xb = Xbuf[xi % NR]
                    xi += 1
                    P.dma(P.qsp, xs[:, 0:k1 - k0, :], xsrc(ti, k0, k1), reads=xbufs_of(ti), writes=[xb])
                    for m in range(nm):
                        for kc in range(k0, k1):
                            last = (kc == KC - 1) or (m == nm - 1 and kc == k1 - 1)
                            P.op(P.pe,
                                 lambda m=m, kc=kc: nc.tensor.matmul(
                                     banks[m], lhsT=wb[:, kc, m * 128:(m + 1) * 128],
                                     rhs=xs[:, kc - k0, :], start=(kc == 0), stop=(kc == KC - 1)),
                                 reads=[Wbuf[g % 2], xb], writes=[bbufs[m]], inc=last)
                epi(g, ti, banks, bbufs)
        P.barrier()


def setup_common(kb):
    P, nc, c = kb.P, kb.nc, kb.c
    es = P.es
    kb.ps = es.enter_context(nc.psum_tensor("ps", [128, 8 * 512], F32))
    kb.bankbuf = [Buf(excl=True) for _ in range(8)]
    kb.bank = lambda i: kb.ps[:, i * 512:(i + 1) * 512]
    kb.ones_bf = P.sb(es, "ones_bf", [128, 128], BF)
    kb.onesb = Buf()
    P.op(P.dve, lambda: nc.vector.memset(kb.ones_bf[:], 1.0), writes=[kb.onesb])
    kb.mask_d = kb.din("coremask", [128, NCORE])
    kb.mask = P.sb(es, "coremask_sb", [128, NCORE], F32)
    kb.maskb = Buf()
    P.dma(P.qsp, kb.mask[:], kb.mask_d[:, :], writes=[kb.maskb])


def new_stream(kb, tag):
    c = kb.c
    ts = [kb.dscr(kb.name(tag), [128, 4 * c.NT], F32) for _ in range(c.NG)]
    return ts, [Buf() for _ in range(c.NG)]


def new_act(kb, tag, KC, dt=BF):
    c = kb.c
    t = kb.dscr(kb.name(tag), [c.NTI, 128, KC * 512], dt)
    return t, [Buf() for _ in range(c.NTI)]


def act_src(t):
    def f(ti, k0, k1):
        return t[ti, :, k0 * 512:k1 * 512].rearrange("p (k t) -> p k t", t=512)
    return f


def norm_phase(kb, H, Hb, gains_d, outs, tag, NTI=None, tok0=0, NTtot=None, router=None):
    P, nc, c = kb.P, kb.nc, kb.c
    KC = c.KC
    TW = 256
    NTtot = c.NT if NTtot is None else NTtot
    nt = (NTtot if NTI is None else NTI * 512) // TW
    with contextlib.ExitStack() as es:
        gs = []
        gb = Buf()
        for i, gd in enumerate(gains_d):
            g = P.sb(es, kb.name(tag + "g"), [128, KC], F32)
            P.dma(P.qsp, g[:], gd[:, :], writes=[gb])
            gs.append(g)
        ht = [P.sb(es, kb.name(tag + "h"), [128, KC, TW], F32) for _ in range(2)]
        htb = [Buf(), Buf()]
        sq = [P.sb(es, kb.name(tag + "sq"), [128, KC, TW], BF) for _ in range(2)]
        sqb = [Buf(), Buf()]
        ot = [[P.sb(es, kb.name(tag + "o"), [128, KC, TW], BF) for _ in range(2)] for _ in outs]
        otb = [[Buf(), Buf()] for _ in outs]
        rs = [P.sb(es, kb.name(tag + "rs"), [128, TW], F32) for _ in range(2)]
        rsb = [Buf(), Buf()]
        if router is not None:
            wr_d, GateB, GateBb = router
            NE = c.NEXP
            wr = P.sb(es, kb.name(tag + "wr"), [128, KC * NE], F32)
            P.dma(P.qsp, wr[:], wr_d[:, :], writes=[gb])
            hn32 = P.sb(es, kb.name(tag + "h32"), [128, KC, TW], F32); hn32b = Buf()
            lg = P.sb(es, kb.name(tag + "lg"), [NE, TW], F32); lgb = Buf()
            gT = P.sb(es, kb.name(tag + "gT"), [NE, TW], F32); gTb = Buf()
            gbt = [P.sb(es, kb.name(tag + "gbt"), [128, TW], F32) for _ in range(2)]; gbtb = [Buf(), Buf()]
            sm = {n: P.sb(es, kb.name(tag + n), [128, NE], F32) for n in ("lt", "eq", "lt2", "sel", "ex", "se", "gts")}
            sc = {n: P.sb(es, kb.name(tag + n), [128, 1], F32) for n in ("m1", "m2", "nm1", "den")}
            smb = Buf()
        for it in range(nt):
            i2 = it % 2
            t0 = tok0 + it * TW
            for g in range(c.NG):
                src = H[g][:, :].rearrange("p (m t) -> p m t", t=NTtot)[:, :, t0:t0 + TW]
                P.dma(P.qsp, ht[i2][:, g * 4:(g + 1) * 4, :], src, reads=[Hb[g]], writes=[htb[i2]])
            P.op(P.act, lambda: nc.scalar.activation(out=sq[i2][:], in_=ht[i2][:], func=AF.Square),
                 reads=[htb[i2]], writes=[sqb[i2]])
            bk = 6 + i2
            for kc in range(KC):
                P.op(P.pe, lambda kc=kc: nc.tensor.matmul(kb.bank(bk)[:, 0:TW], lhsT=kb.ones_bf[:], rhs=sq[i2][:, kc, :],
                                                          start=(kc == 0), stop=(kc == KC - 1)),
                     reads=[kb.onesb, sqb[i2]], writes=[kb.bankbuf[bk]], inc=(kc == KC - 1))
            P.op(P.act, lambda: nc.scalar.activation(out=rs[i2][:], in_=kb.bank(bk)[:, 0:TW], func=AF.Sqrt,
                                                     bias=EPS, scale=1.0 / c.D),
                 reads=[kb.bankbuf[bk]], writes=[rsb[i2]])
            P.op(P.dve, lambda: nc.vector.reciprocal(out=rs[i2][:], in_=rs[i2][:]), reads=[rsb[i2]], writes=[rsb[i2]])
            if router is not None:
                X_ = mybir.AxisListType.X
                for kc in range(KC):
                    P.op(P.dve, lambda kc=kc: nc.vector.scalar_tensor_tensor(
                        out=hn32[:, kc, :], in0=ht[i2][:, kc, :], scalar=gs[0][:, kc:kc + 1], in1=rs[i2][:],
                        op0=ALU.mult, op1=ALU.mult), reads=[htb[i2], rsb[i2], gb], writes=[hn32b])
                for kc in range(KC):
                    P.op(P.pe, lambda kc=kc: nc.tensor.matmul(kb.bank(4)[0:NE, 0:TW], lhsT=wr[:, kc * NE:(kc + 1) * NE], rhs=hn32[:, kc, :],
                                                              start=(kc == 0), stop=(kc == KC - 1)),
                         reads=[gb, hn32b], writes=[kb.bankbuf[4]], inc=(kc == KC - 1))
                P.op(P.act, lambda: nc.scalar.copy(out=lg[:], in_=kb.bank(4)[0:NE, 0:TW]), reads=[kb.bankbuf[4]], writes=[lgb])
                for sbk in range(TW // 128):
                    cs = slice(sbk * 128, (sbk + 1) * 128)
                    P.op(P.pe, lambda: nc.tensor.transpose(kb.bank(5)[:, 0:NE], lg[:, cs], kb.ident[0:NE, 0:NE]),
                         reads=[lgb, kb.cb], writes=[kb.bankbuf[5]])
                    D_ = P.dve
                    P.op(D_, lambda: nc.vector.tensor_copy(out=sm["lt"][:], in_=kb.bank(5)[:, 0:NE]), reads=[kb.bankbuf[5]], writes=[smb])
                    P.op(D_, lambda: nc.vector.reduce_max(out=sc["m1"][:], in_=sm["lt"][:], axis=X_), reads=[smb], writes=[smb])
                    P.op(D_, lambda: nc.vector.tensor_scalar(out=sm["eq"][:], in0=sm["lt"][:], scalar1=sc["m1"][:, 0:1], scalar2=None, op0=ALU.is_equal),
                         reads=[smb], writes=[smb])
                    P.op(D_, lambda: nc.vector.scalar_tensor_tensor(out=sm["lt2"][:], in0=sm["eq"][:], scalar=-1e30, in1=sm["lt"][:],
                                                                    op0=ALU.mult, op1=ALU.add), reads=[smb], writes=[smb])
                    P.op(D_, lambda: nc.vector.reduce_max(out=sc["m2"][:], in_=sm["lt2"][:], axis=X_), reads=[smb], writes=[smb])
                    P.op(D_, lambda: nc.vector.tensor_scalar(out=sm["sel"][:], in0=sm["lt"][:], scalar1=sc["m2"][:, 0:1], scalar2=None, op0=ALU.is_ge),
                         reads=[smb], writes=[smb])
                    P.op(D_, lambda: nc.vector.tensor_scalar(out=sc["nm1"][:], in0=sc["m1"][:], scalar1=-1.0, scalar2=None, op0=ALU.mult),
                         reads=[smb], writes=[smb])
                    P.op(P.act, lambda: nc.scalar.activation(out=sm["ex"][:], in_=sm["lt"][:], func=AF.Exp, bias=sc["nm1"][:, 0:1]),
                         reads=[smb], writes=[smb])
                    P.op(D_, lambda: nc.vector.tensor_tensor(out=sm["se"][:], in0=sm["sel"][:], in1=sm["ex"][:], op=ALU.mult), reads=[smb], writes=[smb])
                    P.op(D_, lambda: nc.vector.reduce_sum(out=sc["den"][:], in_=sm["se"][:], axis=X_), reads=[smb], writes=[smb])
                    P.op(D_, lambda: nc.vector.reciprocal(out=sc["den"][:], in_=sc["den"][:]), reads=[smb], writes=[smb])
                    P.op(D_, lambda: nc.vector.tensor_scalar(out=sm["gts"][:], in0=sm["se"][:], scalar1=sc["den"][:, 0:1], scalar2=None, op0=ALU.mult),
                         reads=[smb], writes=[smb])
                    P.op(P.pe, lambda: nc.tensor.transpose(kb.bank(5)[0:NE, 128:256], sm["gts"][:], kb.ident[:]),
                         reads=[smb, kb.cb], writes=[kb.bankbuf[5]])
                    P.op(P.act, lambda: nc.scalar.copy(out=gT[:, cs], in_=kb.bank(5)[0:NE, 128:256]), reads=[kb.bankbuf[5]], writes=[gTb])
                P.op(P.pe, lambda: nc.tensor.matmul(kb.bank(4)[:, 256:256 + TW], lhsT=kb.selc[:], rhs=gT[:], start=True, stop=True),
                     reads=[kb.cb, gTb], writes=[kb.bankbuf[4]])
                P.op(P.act, lambda: nc.scalar.copy(out=gbt[i2][:], in_=kb.bank(4)[:, 256:256 + TW]), reads=[kb.bankbuf[4]], writes=[gbtb[i2]])
                P.dma(P.qact, GateB[:, t0:t0 + TW], gbt[i2][:], reads=[gbtb[i2]], writes=[GateBb])
            for oi, (OT, OB) in enumerate(outs):
                for kc in range(KC):
                    eng, ee = P.dve, nc.vector
                    P.op(eng, lambda kc=kc, ee=ee: ee.scalar_tensor_tensor(
                        out=ot[oi][i2][:, kc, :], in0=ht[i2][:, kc, :], scalar=gs[oi][:, kc:kc + 1], in1=rs[i2][:],
                        op0=ALU.mult, op1=ALU.mult), reads=[htb[i2], rsb[i2], gb], writes=[otb[oi][i2]])
                ti, half = divmod(it, 2)
                dst = OT[ti, :, :].rearrange("p (k t) -> p k t", t=512)[:, :, half * TW:(half + 1) * TW]
                P.dma(P.qact, dst, ot[oi][i2][:], reads=[otb[oi][i2]], writes=[OB[ti]])
        P.barrier()


def allreduce_stream(kb, Pin, Pinb, Hout, Houtb, g):
    kb.P.allreduce(Pin[g][:, :], Hout[g][:, :], reads=[Pinb[g]], writes=[Houtb[g]])


def scatter_x(kb, xin, Pin, Pinb):
    P, nc, c = kb.P, kb.nc, kb.c
    TB = c.TB
    with contextlib.ExitStack() as es:
        xt = [P.sb(es, kb.name("xs"), [128, 4, TB], F32) for _ in range(2)]
        xtb = [Buf(), Buf()]
        tmp = [P.sb(es, kb.name("xm"), [128, 4, TB], F32) for _ in range(2)]
        tmpb = [Buf(), Buf()]
        k = 0
        for g in range(c.NG):
            P.dma(P.qsp, xt[g % 2][:], xin[g, :, :].rearrange("p (m t) -> p m t", t=TB), writes=[xtb[g % 2]])
            for tb in range(NCORE):
                P.op(P.dve, lambda: nc.vector.tensor_scalar(out=tmp[k % 2][:], in0=xt[g % 2][:], scalar1=kb.mask[:, tb:tb + 1],
                                                            scalar2=None, op0=ALU.mult),
                     reads=[xtb[g % 2], kb.maskb], writes=[tmpb[k % 2]])
                dst = Pin[g][:, :].rearrange("p (m t) -> p m t", t=c.NT)[:, :, tb * TB:(tb + 1) * TB]
                P.dma(P.qact, dst, tmp[k % 2][:], reads=[tmpb[k % 2]], writes=[Pinb[g]])
                k += 1
        P.barrier()


def swiglu_stage1(kb, tag, Wd, G, npairs_of, X, Xb, A, Ab, gate=None):
    P, nc, c = kb.P, kb.nc, kb.c
    with contextlib.ExitStack() as es:
        st = [P.sb(es, kb.name(tag + "s"), [128, 512], F32) for _ in range(2)]
        stb = [Buf(), Buf()]
        at = [P.sb(es, kb.name(tag + "a"), [128, 512], BF) for _ in range(3)]
        atb = [Buf() for _ in range(3)]
        gt = gtb = None
        if gate is not None:
            gt = [P.sb(es, kb.name(tag + "gt"), [128, 512], F32) for _ in range(2)]
            gtb = [Buf(), Buf()]
        cnt = [0]

        def epi(g, ti, banks, bbufs):
            if gate is not None:
                P.dma(P.qact, gt[ti % 2][:], gate[0][:, ti * 512:(ti + 1) * 512], reads=[gate[1]], writes=[gtb[ti % 2]])
            for j in range(npairs_of(g)):
                k = cnt[0]
                cnt[0] += 1
                s, sb_, a, ab = st[k % 2], stb[k % 2], at[k % 3], atb[k % 3]
                P.op(P.act, lambda: nc.scalar.activation(out=s[:], in_=banks[2 * j], func=AF.Silu),
                     reads=[bbufs[2 * j]], writes=[sb_])
                if gate is not None:
                    P.op(P.pool, lambda: nc.gpsimd.tensor_tensor(out=s[:], in0=s[:], in1=gt[ti % 2][:], op=ALU.mult),
                         reads=[sb_, gtb[ti % 2]], writes=[sb_])
                P.op(P.dve, lambda: nc.vector.tensor_tensor(out=a[:], in0=s[:], in1=banks[2 * j + 1], op=ALU.mult),
                     reads=[sb_, bbufs[2 * j + 1]], writes=[ab])
                fch = g * 2 + j
                P.dma(P.qpool, A[ti, :, fch * 512:(fch + 1) * 512], a[:], reads=[ab], writes=[Ab[ti]])

        gemm(kb, tag, Wd, c.KC, G, lambda g: 2 * npairs_of(g), act_src(X), lambda ti: [Xb[ti]], epi, c.NTI)


def out_gemm(kb, tag, Wd, KC, X, Xb, Hprev, Hprevb, Pout, Poutb, Hnew, Hnewb):
    P, nc, c = kb.P, kb.nc, kb.c
    with contextlib.ExitStack() as es:
        rt = [P.sb(es, kb.name(tag + "r"), [128, 4, 512], F32) for _ in range(2)]
        rtb = [Buf(), Buf()]
        ot = [P.sb(es, kb.name(tag + "o"), [128, 4, 512], F32) for _ in range(2)]
        otb = [Buf(), Buf()]
        cnt = [0]

        def epi(g, ti, banks, bbufs):
            k = cnt[0]
            cnt[0] += 1
            r, rb, o, ob = rt[k % 2], rtb[k % 2], ot[k % 2], otb[k % 2]
            src = Hprev[g][:, :].rearrange("p (m t) -> p m t", t=c.NT)[:, :, ti * 512:(ti + 1) * 512]
            P.dma(P.qact, r[:], src, reads=[Hprevb[g]], writes=[rb])
            for m in range(4):
                P.op(P.dve, lambda m=m: nc.vector.scalar_tensor_tensor(
                    out=o[:, m, :], in0=r[:, m, :], scalar=kb.mask[:, 0:1], in1=banks[m], op0=ALU.mult, op1=ALU.add),
                    reads=[rb, kb.maskb, bbufs[m]], writes=[ob])
            dst = Pout[g][:, :].rearrange("p (m t) -> p m t", t=c.NT)[:, :, ti * 512:(ti + 1) * 512]
            P.dma(P.qpool, dst, o[:], reads=[ob], writes=[Poutb[g]])
            if ti == c.NTI - 1:
                allreduce_stream(kb, Pout, Poutb, Hnew, Hnewb, g)

        gemm(kb, tag, Wd, KC, c.NG, lambda g: 4, act_src(X), lambda ti: [Xb[ti]], epi, c.NTI)


def proj_gemm(kb, tag, Wd, G, nm_of, X, Xb, dsts):
    P, nc, c = kb.P, kb.nc, kb.c
    with contextlib.ExitStack() as es:
        t32 = [P.sb(es, kb.name(tag + "t"), [128, 512], F32) for _ in range(3)]
        tbf = [P.sb(es, kb.name(tag + "u"), [128, 512], BF) for _ in range(3)]
        tb = [Buf() for _ in range(3)]
        cnt = [0]

        def epi(g, ti, banks, bbufs):
            for m in range(nm_of(g)):
                ch = g * 4 + m
                if ch >= len(dsts) or dsts[ch] is None:
                    continue
                d, db, scale = dsts[ch]
                k = cnt[0]
                cnt[0] += 1
                t = (t32 if d.dtype == F32 else tbf)[k % 3]
                if k % 2 == 0:
                    P.op(P.act, lambda: nc.scalar.activation(out=t[:], in_=banks[m], func=AF.Copy, scale=float(scale)),
                         reads=[bbufs[m]], writes=[tb[k % 3]])
                else:
                    P.op(P.dve, lambda: nc.vector.tensor_scalar(out=t[:], in0=banks[m], scalar1=float(scale), scalar2=None,
                                                                op0=ALU.mult), reads=[bbufs[m]], writes=[tb[k % 3]])
                P.dma(P.qpool, d[:, ti * 512:(ti + 1) * 512], t[:], reads=[tb[k % 3]], writes=[db])

        gemm(kb, tag, Wd, c.KC, G, nm_of, act_src(X), lambda ti: [Xb[ti]], epi, c.NTI)


def ple_phase(kb, tag, Hin, Hinb, g_d, Wgd, Wc, pin, Hout, Houtb):
    P, nc, c = kb.P, kb.nc, kb.c
    PC = c.PC
    Xp, Xpb = new_act(kb, tag + "xp", c.KC)
    norm_phase(kb, Hin, Hinb, [g_d], [(Xp, Xpb)], tag + "n")
    XT, XTb = new_act(kb, tag + "xt", 2 * PC)
    with contextlib.ExitStack() as es:
        pt = [P.sb(es, kb.name(tag + "p"), [128, PC * 512], F32) for _ in range(2)]
        pb = [P.sb(es, kb.name(tag + "pb"), [128, PC * 512], BF) for _ in range(2)]
        ptb, pbb = [Buf(), Buf()], [Buf(), Buf()]
        for ti in range(c.NTI):
            P.dma(P.qsp, pt[ti % 2][:], pin[ti, :, :], writes=[ptb[ti % 2]])
            P.op(P.dve, lambda: nc.vector.tensor_copy(out=pb[ti % 2][:], in_=pt[ti % 2][:]), reads=[ptb[ti % 2]], writes=[pbb[ti % 2]])
            P.dma(P.qact, XT[ti, :, PC * 512:2 * PC * 512], pb[ti % 2][:], reads=[pbb[ti % 2]], writes=[XTb[ti]])
        tt = [P.sb(es, kb.name(tag + "t1"), [128, 512], BF) for _ in range(3)]
        ttb = [Buf() for _ in range(3)]
        cnt = [0]

        def epi1(g, ti, banks, bbufs):
            for m in range(PC):
                k = cnt[0]
                cnt[0] += 1
                P.op(P.act, lambda: nc.scalar.copy(out=tt[k % 3][:], in_=banks[m]), reads=[bbufs[m]], writes=[ttb[k % 3]])
                P.dma(P.qpool, XT[ti, :, m * 512:(m + 1) * 512], tt[k % 3][:], reads=[ttb[k % 3]], writes=[XTb[ti]])

        gemm(kb, tag + "a", Wgd, c.KC, 1, lambda g: PC, act_src(Xp), lambda ti: [Xpb[ti]], epi1, c.NTI)
    with contextlib.ExitStack() as es:
        sg = [P.sb(es, kb.name(tag + "sg"), [128, 512], F32) for _ in range(2)]
        sgb = [Buf(), Buf()]
        rt = [P.sb(es, kb.name(tag + "r"), [128, 2, 512], F32) for _ in range(2)]
        rtb = [Buf(), Buf()]
        ot = [P.sb(es, kb.name(tag + "o"), [128, 2, 512], F32) for _ in range(2)]
        otb = [Buf(), Buf()]
        cnt2 = [0, 0]

        def epi2(gg, ti, banks, bbufs):
            g, mc0 = gg // 2, (gg % 2) * 2
            k = cnt2[0]
            cnt2[0] += 1
            r, rb, o, ob = rt[k % 2], rtb[k % 2], ot[k % 2], otb[k % 2]
            src = Hin[g][:, :].rearrange("p (m t) -> p m t", t=c.NT)[:, mc0:mc0 + 2, ti * 512:(ti + 1) * 512]
            P.dma(P.qact, r[:], src, reads=[Hinb[g]], writes=[rb])
            for j in range(2):
                k2 = cnt2[1]
                cnt2[1] += 1
                s, sb_ = sg[k2 % 2], sgb[k2 % 2]
                P.op(P.act, lambda: nc.scalar.activation(out=s[:], in_=banks[2 * j], func=AF.Sigmoid),
                     reads=[bbufs[2 * j]], writes=[sb_])
                P.op(P.dve, lambda: nc.vector.tensor_tensor(out=s[:], in0=s[:], in1=banks[2 * j + 1], op=ALU.mult),
                     reads=[sb_, bbufs[2 * j + 1]], writes=[sb_])
                P.op(P.pool, lambda: nc.gpsimd.tensor_tensor(out=o[:, j, :], in0=s[:], in1=r[:, j, :], op=ALU.add),
                     reads=[sb_, rb], writes=[ob])
            dst = Hout[g][:, :].rearrange("p (m t) -> p m t", t=c.NT)[:, mc0:mc0 + 2, ti * 512:(ti + 1) * 512]
            P.dma(P.qpool, dst, o[:], reads=[ob], writes=[Houtb[g]])

        gemm(kb, tag + "b", Wc, 2 * PC, 2 * c.NG, lambda g: 4, act_src(XT), lambda ti: [XTb[ti]], epi2, c.NTI)


def stream_layout(hT, NT):
    D = hT.shape[0]
    NG = D // 512
    return np.ascontiguousarray(hT.reshape(NG, 4, 128, NT).transpose(0, 2, 1, 3).reshape(NG, 128, 4 * NT))


def stream_unlayout(a, NT):
    NG = a.shape[0]
    return a.reshape(NG, 128, 4, NT).transpose(0, 2, 1, 3).reshape(NG * 512, NT)


def gain_layout(g):
    return np.ascontiguousarray(g.reshape(-1, 128).T)


def tile_layout(xT):
    Kd, NT = xT.shape
    KC = Kd // 128
    return np.ascontiguousarray(xT.reshape(KC, 128, NT // 512, 512).transpose(2, 1, 0, 3).reshape(NT // 512, 128, KC * 512))


def ple_combined(w_gup, w_up):
    PL, D = w_gup.shape
    Wc = np.zeros((2 * PL, 2 * D), np.float32)
    for m in range(D // 128):
        Wc[:PL, (2 * m) * 128:(2 * m + 1) * 128] = w_gup[:, m * 128:(m + 1) * 128]
        Wc[PL:, (2 * m + 1) * 128:(2 * m + 2) * 128] = w_up[:, m * 128:(m + 1) * 128]
    return group_weights(Wc)


def load_consts(kb):
    P, nc, c = kb.P, kb.nc, kb.c
    es = P.es
    kb.cb = Buf()
    def ld(name, shape):
        d = kb.din(name, shape)
        t = P.sb(es, name + "_sb", shape, F32)
        P.dma(P.qsp, t[:], d[:, :], writes=[kb.cb])
        return t
    kb.ident = ld("c_ident", [128, 128])
    kb.rotm = ld("c_rotm", [128, 128])
    kb.tri = ld("c_tri", [128, 128])
    kb.ones32 = ld("c_ones", [128, 128])
    kb.mask2 = ld("c_mask2", [128, 256])
    kb.sbmask = ld("c_sbmask", [128, 4 * 512])
    kb.selc = ld("c_selc", [NCORE, 128])


def host_consts(c, core):
    i = np.arange(128)
    rot = np.zeros((128, 128), np.float32)
    rot[i[:64] + 64, i[:64]] = -1.0
    rot[i[64:] - 64, i[64:]] = 1.0
    tri = (i[:, None] >= i[None, :]).astype(np.float32)
    mask2 = np.zeros((128, 256), np.float32)
    mask2[:, :128] = (i[:, None] >= i[None, :])
    mask2[:, 128:] = (i[:, None] <= i[None, :])
    q = np.arange(512)
    sbm = np.concatenate([((d * 128 + i[:, None]) < q[None, :]).astype(np.float32) for d in range(4)], axis=1)
    selc = np.zeros((NCORE, 128), np.float32)
    selc[core] = 1.0
    half = 64
    inv = (10000.0 ** (-np.arange(half, dtype=np.float32) / half)).astype(np.float32)
    ang = np.arange(c.S, dtype=np.float32)[None, :] * np.concatenate([inv, inv])[:, None]
    return {"c_ident": np.eye(128, dtype=np.float32), "c_rotm": rot, "c_tri": tri, "c_ones": np.ones((128, 128), np.float32),
            "c_mask2": mask2, "c_sbmask": np.ascontiguousarray(sbm), "c_selc": selc,
            "c_cos": np.cos(ang).astype(np.float32), "c_sin": np.sin(ang).astype(np.float32)}


def attn0_phase(kb, qkv0, qkvb, gq_d, gk_d, O0, O0b):
    P, nc, c = kb.P, kb.nc, kb.c
    S = c.S
    NS = S // 512
    scale = 1.0 / np.sqrt(128.0)
    cos_d = kb.din("c_cos", [128, S])
    sin_d = kb.din("c_sin", [128, S])
    with contextlib.ExitStack() as es:
        cb = Buf()
        cosT = P.sb(es, "cosT", [128, S], F32)
        sinT = P.sb(es, "sinT", [128, S], F32)
        gq = P.sb(es, "gq", [128, 1], F32)
        gk = P.sb(es, "gk", [128, 1], F32)
        P.dma(P.qsp, cosT[:], cos_d[:, :], writes=[cb])
        P.dma(P.qsp, sinT[:], sin_d[:, :], writes=[cb])
        P.dma(P.qsp, gq[:], gq_d[:, :], writes=[cb])
        P.dma(P.qsp, gk[:], gk_d[:, :], writes=[cb])
        vraw = P.sb(es, "a0vraw", [128, S], F32); vrawb = Buf()
        raw = [P.sb(es, kb.name("a0raw"), [128, 512], F32) for _ in range(3)]; rawb = [Buf() for _ in range(3)]
        sq = [P.sb(es, kb.name("a0sq"), [128, 512], BF) for _ in range(2)]; sqb = [Buf(), Buf()]
        rs = [P.sb(es, kb.name("a0rs"), [128, 512], F32) for _ in range(2)]; rsb = [Buf(), Buf()]
        qn = [P.sb(es, kb.name("a0qn"), [128, 512], F32) for _ in range(2)]; qnb = [Buf(), Buf()]
        t1 = [P.sb(es, kb.name("a0t1"), [128, 512], F32) for _ in range(2)]; t1b = [Buf(), Buf()]
        t2 = [P.sb(es, kb.name("a0t2"), [128, 512], F32) for _ in range(2)]; t2b = [Buf(), Buf()]
        qf = P.sb(es, "a0qf", [128, S], BF); qfb = Buf()
        kf = P.sb(es, "a0kf", [128, S], BF); kfb = Buf()
        vtok = P.sb(es, "a0vtok", [128, S // 128, 128], BF); vtokb = Buf()
        accO = P.sb(es, "a0accO", [128, S], F32); accOb = Buf()
        accL = P.sb(es, "a0accL", [128, S], F32); accLb = Buf()
        pe_ = [P.sb(es, kb.name("a0pe"), [128, 256], F32) for _ in range(2)]; peb = [Buf(), Buf()]
        pm = [P.sb(es, kb.name("a0pm"), [128, 256], BF) for _ in range(2)]; pmb = [Buf(), Buf()]
        k3 = 0
        k2 = 0
        ka = 0
        for hl in range(c.AHC):
            for b in range(c.B):
                for gr, (span, dil) in enumerate(c.groups):
                    u = gr * c.AHC + hl
                    for which, (dst, dstb, gain) in enumerate(((qf, qfb, gq), (kf, kfb, gk))):
                        ch = 3 * u + which
                        for st in range(NS):
                            r_, rb_ = raw[k3 % 3], rawb[k3 % 3]
                            k3 += 1
                            i2 = k2 % 2
                            k2 += 1
                            cs = slice(st * 512, (st + 1) * 512)
                            P.dma(P.qsp, r_[:], qkv0[ch, :, b * S + st * 512: b * S + (st + 1) * 512], reads=[qkvb], writes=[rb_])
                            P.op(P.act, lambda: nc.scalar.activation(out=sq[i2][:], in_=r_[:], func=AF.Square), reads=[rb_], writes=[sqb[i2]])
                            bk = 6 + i2
                            P.op(P.pe, lambda: nc.tensor.matmul(kb.bank(bk), lhsT=kb.ones_bf[:], rhs=sq[i2][:], start=True, stop=True),
                                 reads=[kb.onesb, sqb[i2]], writes=[kb.bankbuf[bk]])
                            P.op(P.act, lambda: nc.scalar.activation(out=rs[i2][:], in_=kb.bank(bk), func=AF.Sqrt, bias=EPS, scale=1.0 / 128),
                                 reads=[kb.bankbuf[bk]], writes=[rsb[i2]])
                            P.op(P.dve, lambda: nc.vector.reciprocal(out=rs[i2][:], in_=rs[i2][:]), reads=[rsb[i2]], writes=[rsb[i2]])
                            P.op(P.dve, lambda: nc.vector.scalar_tensor_tensor(out=qn[i2][:], in0=r_[:], scalar=gain[:, 0:1], in1=rs[i2][:],
                                                                                op0=ALU.mult, op1=ALU.mult),
                                 reads=[rb_, rsb[i2], cb], writes=[qnb[i2]])
                            bk2 = 4 + i2
                            P.op(P.pe, lambda: nc.tensor.matmul(kb.bank(bk2), lhsT=kb.rotm[:], rhs=qn[i2][:], start=True, stop=True),
                                 reads=[kb.cb, qnb[i2]], writes=[kb.bankbuf[bk2]])
                            P.op(P.pool, lambda: nc.gpsimd.tensor_tensor(out=t1[i2][:], in0=qn[i2][:], in1=cosT[:, cs], op=ALU.mult),
                                 reads=[qnb[i2], cb], writes=[t1b[i2]])
                            P.op(P.dve, lambda: nc.vector.tensor_tensor(out=t2[i2][:], in0=sinT[:, cs], in1=kb.bank(bk2), op=ALU.mult),
                                 reads=[kb.bankbuf[bk2], cb], writes=[t2b[i2]])
                            P.op(P.pool, lambda: nc.gpsimd.tensor_tensor(out=dst[:, cs], in0=t1[i2][:], in1=t2[i2][:], op=ALU.add),
                                 reads=[t1b[i2], t2b[i2]], writes=[dstb])
                    P.dma(P.qsp, vraw[:], qkv0[3 * u + 2, :, b * S:(b + 1) * S], reads=[qkvb], writes=[vrawb])
                    L = S // dil
                    nbk = L // 128
                    for r in range(dil):
                        for n in range(nbk):
                            cols = slice(r + n * 128 * dil, r + n * 128 * dil + 127 * dil + 1, dil)
                            P.op(P.pe, lambda: nc.tensor.transpose(kb.bank(3)[:, 0:128], vraw[:, cols], kb.ident[:]),
                                 reads=[vrawb, kb.cb], writes=[kb.bankbuf[3]])
                            P.op(P.act, lambda: nc.scalar.copy(out=vtok[:, r * nbk + n, :], in_=kb.bank(3)[:, 0:128]),
                                 reads=[kb.bankbuf[3]], writes=[vtokb])
                    for r in range(dil):
                        for n in range(nbk):
                            i2 = ka % 2
                            ka += 1
                            qcols = slice(r + n * 128 * dil, r + n * 128 * dil + 127 * dil + 1, dil)
                            kbs = [n - 1, n] if n > 0 else [n]
                            sbk, olb = i2, 2 + i2
                            for kbk in kbs:
                                reg = slice(128, 256) if kbk == n else slice(0, 128)
                                kcols = slice(r + kbk * 128 * dil, r + kbk * 128 * dil + 127 * dil + 1, dil)
                                P.op(P.pe, lambda: nc.tensor.matmul(kb.bank(sbk)[:, reg], lhsT=kf[:, kcols], rhs=qf[:, qcols], start=True, stop=True),
                                     reads=[kfb, qfb], writes=[kb.bankbuf[sbk]])
                            lo = 0 if n > 0 else 128
                            P.op(P.act, lambda: nc.scalar.activation(out=pe_[i2][:, lo:256], in_=kb.bank(sbk)[:, lo:256], func=AF.Exp, scale=float(scale)),
                                 reads=[kb.bankbuf[sbk]], writes=[peb[i2]])
                            P.op(P.dve, lambda: nc.vector.tensor_tensor(out=pm[i2][:, lo:256], in0=pe_[i2][:, lo:256], in1=kb.mask2[:, lo:256], op=ALU.mult),
                                 reads=[peb[i2], kb.cb], writes=[pmb[i2]])
                            for idx, kbk in enumerate(kbs):
                                reg = slice(128, 256) if kbk == n else slice(0, 128)
                                P.op(P.pe, lambda: nc.tensor.matmul(kb.bank(olb)[:, 0:128], lhsT=vtok[:, r * nbk + kbk, :], rhs=pm[i2][:, reg],
                                                                    start=(idx == 0), stop=(idx == len(kbs) - 1)),
                                     reads=[vtokb, pmb[i2]], writes=[kb.bankbuf[olb]], inc=False)
                            for idx, kbk in enumerate(kbs):
                                reg = slice(128, 256) if kbk == n else slice(0, 128)
                                P.op(P.pe, lambda: nc.tensor.matmul(kb.bank(olb)[:, 128:256], lhsT=kb.ones_bf[:], rhs=pm[i2][:, reg],
                                                                    start=(idx == 0), stop=(idx == len(kbs) - 1)),
                                     reads=[kb.onesb, pmb[i2]], writes=[kb.bankbuf[olb]], inc=(idx == len(kbs) - 1))
                            if gr == 0:
                                P.op(P.act, lambda: nc.scalar.copy(out=accO[:, qcols], in_=kb.bank(olb)[:, 0:128]), reads=[kb.bankbuf[olb]], writes=[accOb])
                                P.op(P.dve, lambda: nc.vector.tensor_copy(out=accL[:, qcols], in_=kb.bank(olb)[:, 128:256]), reads=[kb.bankbuf[olb]], writes=[accLb])
                            else:
                                P.op(P.dve, lambda: nc.vector.tensor_tensor(out=accO[:, qcols], in0=accO[:, qcols], in1=kb.bank(olb)[:, 0:128], op=ALU.add),
                                     reads=[kb.bankbuf[olb], accOb], writes=[accOb])
                                P.op(P.dve, lambda: nc.vector.tensor_tensor(out=accL[:, qcols], in0=accL[:, qcols], in1=kb.bank(olb)[:, 128:256], op=ALU.add),
                                     reads=[kb.bankbuf[olb], accLb], writes=[accLb])
                P.op(P.dve, lambda: nc.vector.reciprocal(out=accL[:], in_=accL[:]), reads=[accLb], writes=[accLb])
                P.op(P.dve, lambda: nc.vector.tensor_tensor(out=qf[:], in0=accO[:], in1=accL[:], op=ALU.mult), reads=[accOb, accLb], writes=[qfb])
                ti0 = b * NS
                for st in range(NS):
                    P.dma(P.qsp, O0[ti0 + st, :, hl * 512:(hl + 1) * 512], qf[:, st * 512:(st + 1) * 512], reads=[qfb], writes=[O0b[ti0 + st]])
        P.barrier()


def attn1_phase(kb, Q1, K1, V1, qkvb, O1, O1b):
    P, nc, c = kb.P, kb.nc, kb.c
    S = c.S
    NS = S // 512
    with contextlib.ExitStack() as es:
        qf = P.sb(es, "a1qf", [128, S], BF); qfb = Buf()
        kf = P.sb(es, "a1kf", [128, S], BF); kfb = Buf()
        vraw = P.sb(es, "a1vraw", [128, S], F32); vrawb = Buf()
        vtok = P.sb(es, "a1vtok", [128, S // 128, 128], BF); vtokb = Buf()
        e_ = [P.sb(es, kb.name("a1e"), [128, 512], F32) for _ in range(2)]; eb = [Buf(), Buf()]
        sp = [P.sb(es, kb.name("a1sp"), [128, 512], F32) for _ in range(2)]; spb = [Buf(), Buf()]
        ec = [P.sb(es, kb.name("a1ec"), [128, 512], F32) for _ in range(2)]; ecb = [Buf(), Buf()]
        a32 = [P.sb(es, kb.name("a1a32"), [128, 512], F32) for _ in range(2)]; a32b = [Buf(), Buf()]
        a_ = [P.sb(es, kb.name("a1a"), [128, 512], BF) for _ in range(2)]; ab = [Buf(), Buf()]
        sacc = P.sb(es, "a1sacc", [128, 512], F32); saccb = Buf()
        ob = [P.sb(es, kb.name("a1ob"), [128, 512], BF) for _ in range(2)]; obb = [Buf(), Buf()]
        kk = 0
        for j in range(c.BHC):
            for b in range(c.B):
                P.dma(P.qsp, qf[:], Q1[j][:, b * S:(b + 1) * S], reads=[qkvb], writes=[qfb])
                P.dma(P.qsp, kf[:], K1[j][:, b * S:(b + 1) * S], reads=[qkvb], writes=[kfb])
                P.dma(P.qsp, vraw[:], V1[j][:, b * S:(b + 1) * S], reads=[qkvb], writes=[vrawb])
                for blk in range(S // 128):
                    P.op(P.pe, lambda: nc.tensor.transpose(kb.bank(6)[:, 0:128], vraw[:, blk * 128:(blk + 1) * 128], kb.ident[:]),
                         reads=[vrawb, kb.cb], writes=[kb.bankbuf[6]])
                    P.op(P.dve, lambda: nc.vector.tensor_copy(out=vtok[:, blk, :], in_=kb.bank(6)[:, 0:128]), reads=[kb.bankbuf[6]], writes=[vtokb])
                for qt in range(NS):
                    q0 = qt * 512
                    hi = q0 // 128 + 3
                    obk = 4 + qt % 2
                    first = True
                    for kbk in range(hi, -1, -1):
                        i2 = kk % 2
                        kk += 1
                        delta = kbk * 128 - q0
                        zb, cbk = i2, 2 + i2
                        P.op(P.pe, lambda: nc.tensor.matmul(kb.bank(zb), lhsT=kf[:, kbk * 128:(kbk + 1) * 128], rhs=qf[:, q0:q0 + 512], start=True, stop=True),
                             reads=[kfb, qfb], writes=[kb.bankbuf[zb]])
                        P.op(P.act, lambda: nc.scalar.activation(out=e_[i2][:], in_=kb.bank(zb), func=AF.Exp), reads=[kb.bankbuf[zb]], writes=[eb[i2]])
                        P.op(P.act, lambda: nc.scalar.activation(out=sp[i2][:], in_=e_[i2][:], func=AF.Ln, bias=1.0), reads=[eb[i2]], writes=[spb[i2]])
                        if delta >= 0:
                            mk = kb.sbmask[:, (delta // 128) * 512:(delta // 128 + 1) * 512]
                            P.op(P.pool, lambda: nc.gpsimd.tensor_tensor(out=sp[i2][:], in0=sp[i2][:], in1=mk, op=ALU.mult),
                                 reads=[spb[i2], kb.cb], writes=[spb[i2]])
                        P.op(P.pe, lambda: nc.tensor.matmul(kb.bank(cbk), lhsT=kb.tri[:], rhs=sp[i2][:], start=True, stop=first),
                             reads=[kb.cb, spb[i2]], writes=[kb.bankbuf[cbk]], inc=first)
                        if not first:
                            P.op(P.pe, lambda: nc.tensor.matmul(kb.bank(cbk), lhsT=kb.ones32[:], rhs=sacc[:], start=False, stop=True),
                                 reads=[kb.cb, saccb], writes=[kb.bankbuf[cbk]])
                            P.op(P.pool, lambda: nc.gpsimd.tensor_tensor(out=sacc[:], in0=sacc[:], in1=sp[i2][:], op=ALU.add),
                                 reads=[saccb, spb[i2]], writes=[saccb])
                        else:
                            P.op(P.pool, lambda: nc.gpsimd.tensor_copy(out=sacc[:], in_=sp[i2][:]), reads=[spb[i2]], writes=[saccb])
                        P.op(P.act, lambda: nc.scalar.activation(out=ec[i2][:], in_=kb.bank(cbk), func=AF.Exp, scale=-1.0),
                             reads=[kb.bankbuf[cbk]], writes=[ecb[i2]])
                        if delta >= 0:
                            P.op(P.dve, lambda: nc.vector.tensor_tensor(out=a32[i2][:], in0=e_[i2][:], in1=ec[i2][:], op=ALU.mult),
                                 reads=[eb[i2], ecb[i2]], writes=[a32b[i2]])
                            P.op(P.dve, lambda: nc.vector.tensor_tensor(out=a_[i2][:], in0=a32[i2][:], in1=mk, op=ALU.mult),
                                 reads=[a32b[i2], kb.cb], writes=[ab[i2]])
                        else:
                            P.op(P.dve, lambda: nc.vector.tensor_tensor(out=a_[i2][:], in0=e_[i2][:], in1=ec[i2][:], op=ALU.mult),
                                 reads=[eb[i2], ecb[i2]], writes=[ab[i2]])
                        P.op(P.pe, lambda: nc.tensor.matmul(kb.bank(obk), lhsT=vtok[:, kbk, :], rhs=a_[i2][:], start=first, stop=(kbk == 0)),
                             reads=[vtokb, ab[i2]], writes=[kb.bankbuf[obk]])
                        first = False
                    o2 = qt % 2
                    P.op(P.act, lambda: nc.scalar.copy(out=ob[o2][:], in_=kb.bank(obk)), reads=[kb.bankbuf[obk]], writes=[obb[o2]])
                    P.dma(P.qsp, O1[b * NS + qt, :, j * 512:(j + 1) * 512], ob[o2][:], reads=[obb[o2]], writes=[O1b[b * NS + qt]])
        P.barrier()


def select_own(kb, H, Hb, out):
    P, nc, c = kb.P, kb.nc, kb.c
    TB = c.TB
    with contextlib.ExitStack() as es:
        blk = [P.sb(es, kb.name("sblk"), [128, 4, TB], F32) for _ in range(2)]; blkb = [Buf(), Buf()]
        acc = [P.sb(es, kb.name("sacc"), [128, 4, TB], F32) for _ in range(2)]; accb = [Buf(), Buf()]
        k = 0
        ob = Buf()
        for g in range(c.NG):
            a, ab = acc[g % 2], accb[g % 2]
            for tb in range(NCORE):
                bl, bb = blk[k % 2], blkb[k % 2]
                k += 1
                src = H[g][:, :].rearrange("p (m t) -> p m t", t=c.NT)[:, :, tb * TB:(tb + 1) * TB]
                P.dma(P.qsp, bl[:], src, reads=[Hb[g]], writes=[bb])
                if tb == 0:
                    P.op(P.dve, lambda: nc.vector.tensor_scalar(out=a[:], in0=bl[:], scalar1=kb.mask[:, 0:1], scalar2=None, op0=ALU.mult),
                         reads=[bb, kb.maskb], writes=[ab])
                else:
                    P.op(P.dve, lambda: nc.vector.scalar_tensor_tensor(out=a[:], in0=bl[:], scalar=kb.mask[:, tb:tb + 1], in1=a[:],
                                                                       op0=ALU.mult, op1=ALU.add), reads=[bb, kb.maskb, ab], writes=[ab])
            P.dma(P.qact, out[g, :, :], a[:].rearrange("p m t -> p (m t)"), reads=[ab], writes=[ob])
        P.barrier()


def build_full(c, stop_after=None):
    kb = K(c)
    P = kb.P
    setup_common(kb)
    load_consts(kb)
    D, KC, NG, NT = c.D, c.KC, c.NG, c.NT
    din = kb.din
    xin = din("xT", [NG, 128, 4 * c.TB])
    g_mix = [din("g_mix%d" % i, [128, KC]) for i in range(2)]
    g_ffn = [din("g_ffn%d" % i, [128, KC]) for i in range(2)]
    g_ple = [din("g_ple%d" % i, [128, KC]) for i in range(2)]
    g_kv = din("g_kv", [128, KC])
    gq = din("g_q", [128, 1])
    gk = din("g_k", [128, 1])
    NCH = 3 * c.NGR * c.AHC
    GQ = -(-NCH // 4)
    Wqkv = din("Wqkv", [GQ, 128, KC, 512])
    Wo0 = din("Wo0", [NG, 128, c.AHC, 512])
    G1 = -(-c.FC // 2)
    Wf1 = din("Wf1", [G1, 128, KC, 512])
    Wf2 = din("Wf2", [NG, 128, c.FC, 512])
    Wgd = [din("Wgd%d" % i, [1, 128, KC, 512]) for i in range(2)]
    Wpc = [din("Wpc%d" % i, [2 * NG, 128, 2 * c.PC, 512]) for i in range(2)]
    pin = [din("pin%d" % i, [c.NTI, 128, c.PC * 512]) for i in range(2)]
    GKV = -(-2 * c.BHC // 4)
    Wkv = din("Wkv", [GKV, 128, KC, 512])
    GQ1 = -(-c.BHC // 4)
    Wq1 = din("Wq1", [GQ1, 128, KC, 512])
    Wo1 = din("Wo1", [NG, 128, c.BHC, 512])
    wr_d = din("wr", [128, KC * c.NEXP])
    GM1 = c.EC // 2
    Wm1 = din("Wm1", [GM1, 128, KC, 512])
    Wm2 = din("Wm2", [NG, 128, c.EC, 512])
    out = kb.nc.dram_tensor("out", [NG, 128, 4 * c.TB], F32, kind="ExternalOutput")

    def dbg_out(H, Hb):
        o = kb.nc.dram_tensor("dbg", [NG, 128, 4 * NT], F32, kind="ExternalOutput")
        for g in range(NG):
            P.dma(P.qsp, o[g, :, :], H[g][:, :], reads=[Hb[g]], writes=[Buf()])
        return kb, P.finish()

    Pin, Pinb = new_stream(kb, "Pin"); H0, H0b = new_stream(kb, "H0")
    scatter_x(kb, xin, Pin, Pinb)
    for g in range(NG):
        allreduce_stream(kb, Pin, Pinb, H0, H0b, g)
    X, Xb = new_act(kb, "hn0", KC)
    norm_phase(kb, H0, H0b, [g_mix[0]], [(X, Xb)], "nm0")
    qkv0 = kb.dscr("qkv0", [NCH, 128, NT], F32); qkvb = Buf()
    class _V:
        def __init__(s, t, i): s.t, s.i, s.dtype = t, i, F32
        def __getitem__(s, idx): return s.t[(s.i,) + idx]
    proj_gemm(kb, "qkv", Wqkv, GQ, lambda g: min(4, NCH - 4 * g), X, Xb, [(_V(qkv0, ch), qkvb, 1.0) for ch in range(NCH)])
    if stop_after == "qkv":
        return dbg_out(H0, H0b)
    O0, O0b = new_act(kb, "O0", c.AHC)
    attn0_phase(kb, qkv0, qkvb, gq, gk, O0, O0b)
    if stop_after == "attn0core":
        return dbg_out(H0, H0b)
    P1, P1b = new_stream(kb, "P1"); H1, H1b = new_stream(kb, "H1")
    out_gemm(kb, "wo0", Wo0, c.AHC, O0, O0b, H0, H0b, P1, P1b, H1, H1b)
    if stop_after == "attn0":
        return dbg_out(H1, H1b)
    X, Xb = new_act(kb, "hn1", KC)
    norm_phase(kb, H1, H1b, [g_ffn[0]], [(X, Xb)], "nf0")
    A, Ab = new_act(kb, "A0", c.FC)
    swiglu_stage1(kb, "f1", Wf1, G1, lambda g: min(2, c.FC - 2 * g), X, Xb, A, Ab)
    P2, P2b = new_stream(kb, "P2"); H2, H2b = new_stream(kb, "H2")
    out_gemm(kb, "f2", Wf2, c.FC, A, Ab, H1, H1b, P2, P2b, H2, H2b)
    H3, H3b = new_stream(kb, "H3")
    ple_phase(kb, "pl0", H2, H2b, g_ple[0], Wgd[0], Wpc[0], pin[0], H3, H3b)
    if stop_after == "layer0":
        return dbg_out(H3, H3b)
    Xkv, Xkvb = new_act(kb, "hnkv", KC); Xq, Xqb = new_act(kb, "hnq", KC)
    norm_phase(kb, H3, H3b, [g_kv, g_mix[1]], [(Xkv, Xkvb), (Xq, Xqb)], "nm1")
    K1 = [kb.dscr("K1_%d" % j, [128, NT], BF) for j in range(c.BHC)]
    V1 = [kb.dscr("V1_%d" % j, [128, NT], F32) for j in range(c.BHC)]
    Q1 = [kb.dscr("Q1_%d" % j, [128, NT], BF) for j in range(c.BHC)]
    q1b = Buf()
    proj_gemm(kb, "kv", Wkv, GKV, lambda g: min(4, 2 * c.BHC - 4 * g), Xkv, Xkvb,
              [(K1[j], q1b, 1.0) for j in range(c.BHC)] + [(V1[j], q1b, 1.0) for j in range(c.BHC)])
    proj_gemm(kb, "q1", Wq1, GQ1, lambda g: min(4, c.BHC - 4 * g), Xq, Xqb, [(Q1[j], q1b, 1.0 / np.sqrt(128.0)) for j in range(c.BHC)])
    if stop_after == "proj1":
        return dbg_out(H3, H3b)
    O1, O1b = new_act(kb, "O1", c.BHC)
    attn1_phase(kb, Q1, K1, V1, q1b, O1, O1b)
    if stop_after == "attn1core":
        return dbg_out(H3, H3b)
    P4, P4b = new_stream(kb, "P4"); H4, H4b = new_stream(kb, "H4")
    out_gemm(kb, "wo1", Wo1, c.BHC, O1, O1b, H3, H3b, P4, P4b, H4, H4b)
    if stop_after == "attn1":
        return dbg_out(H4, H4b)
    Xm, Xmb = new_act(kb, "hnm", KC)
    GateB = kb.dscr("GateB", [128, NT], F32); GateBb = Buf()
    norm_phase(kb, H4, H4b, [g_ffn[1]], [(Xm, Xmb)], "nf1", router=(wr_d, GateB, GateBb))
    Am, Amb = new_act(kb, "Am", c.EC)
    swiglu_stage1(kb, "m1", Wm1, GM1, lambda g: 2, Xm, Xmb, Am, Amb, gate=(GateB, GateBb))
    P5, P5b = new_stream(kb, "P5"); H5, H5b = new_stream(kb, "H5")
    out_gemm(kb, "m2", Wm2, c.EC, Am, Amb, H4, H4b, P5, P5b, H5, H5b)
    H6, H6b = new_stream(kb, "H6")
    ple_phase(kb, "pl1", H5, H5b, g_ple[1], Wgd[1], Wpc[1], pin[1], H6, H6b)
    if stop_after == "all_dbg":
        return dbg_out(H6, H6b)
    select_own(kb, H6, H6b, out)
    return kb, P.finish()


def pair_interleave(wg, wu, nchunk):
    Kd = wg.shape[0]
    w = np.empty((Kd, nchunk, 2, 128), np.float32)
    w[:, :, 0, :] = wg.reshape(Kd, nchunk, 128)
    w[:, :, 1, :] = wu.reshape(Kd, nchunk, 128)
    return w.reshape(Kd, nchunk * 256)


def pad_cols(w, n):
    o = np.zeros((w.shape[0], n), np.float32)
    o[:, :w.shape[1]] = w
    return o


def pad_rows(w, n):
    o = np.zeros((n, w.shape[1]), np.float32)
    o[:w.shape[0]] = w
    return o


def prep_inputs(c, I, core):
    D, NT, TB = c.D, c.NT, c.TB
    f = lambda a: np.asarray(a, dtype=np.float32)
    m = dict(host_consts(c, core))
    mask = np.zeros((128, NCORE), np.float32)
    mask[:, core] = 1.0
    m["coremask"] = mask
    xT = f(I["x"]).reshape(NT, D)[core * TB:(core + 1) * TB].T
    m["xT"] = stream_layout(np.ascontiguousarray(xT), TB)
    for i in range(2):
        m["g_mix%d" % i] = gain_layout(f(I["norm_mix"])[i])
        m["g_ffn%d" % i] = gain_layout(f(I["norm_ffn"])[i])
        m["g_ple%d" % i] = gain_layout(f(I["norm_ple"])[i])
        m["Wgd%d" % i] = group_weights(f(I["ple_w_gdown"])[i])
        m["Wpc%d" % i] = ple_combined(f(I["ple_w_gup"])[i], f(I["ple_w_up"])[i])
        m["pin%d" % i] = tile_layout(np.ascontiguousarray(f(I["p"])[i].reshape(NT, c.PLE).T))
    m["g_kv"] = gain_layout(f(I["kv_norm"]))
    m["g_q"] = np.ascontiguousarray(f(I["a_q_norm"])[0].reshape(128, 1))
    m["g_k"] = np.ascontiguousarray(f(I["a_k_norm"])[0].reshape(128, 1))
    wq = f(I["a_w_qkv"])[0].reshape(D, 3, c.NGR, c.AH, 128)
    cols = []
    for gr in range(c.NGR):
        for hl in range(c.AHC):
            h = core * c.AHC + hl
            for t in range(3):
                cols.append(wq[:, t, gr, h, :])
    m["Wqkv"] = group_weights(np.concatenate(cols, axis=1))
    wo = f(I["a_w_o"])[0].reshape(c.AH, 128, D)
    m["Wo0"] = group_weights(wo[core * c.AHC:(core + 1) * c.AHC].reshape(c.AHC * 128, D))
    FS, FC = c.FS, c.FC
    sl = slice(core * FS, (core + 1) * FS)
    wg = pad_cols(f(I["ffn_w_gate"])[0][:, sl], FC * 128)
    wu = pad_cols(f(I["ffn_w_up"])[0][:, sl], FC * 128)
    m["Wf1"] = group_weights(pair_interleave(wg, wu, FC))
    m["Wf2"] = group_weights(pad_rows(f(I["ffn_w_down"])[0][sl], FC * 128))
    wkv = f(I["w_kv"]).reshape(D, 2, c.BH, 128)
    hs = slice(core * c.BHC, (core + 1) * c.BHC)
    m["Wkv"] = group_weights(np.concatenate([wkv[:, 0, hs].reshape(D, -1), wkv[:, 1, hs].reshape(D, -1)], axis=1))
    m["Wq1"] = group_weights(f(I["b_w_q"])[0].reshape(D, c.BH, 128)[:, hs].reshape(D, -1))
    m["Wo1"] = group_weights(f(I["b_w_o"])[0].reshape(c.BH, 128, D)[hs].reshape(c.BHC * 128, D))
    wr = f(I["moe_w_router"])[0]
    m["wr"] = np.ascontiguousarray(wr.reshape(c.KC, 128, c.NEXP).transpose(1, 0, 2).reshape(128, c.KC * c.NEXP))
    e = core
    m["Wm1"] = group_weights(pair_interleave(f(I["moe_w_gate"])[0, e], f(I["moe_w_up"])[0, e], c.EC))
    m["Wm2"] = group_weights(f(I["moe_w_down"])[0, e])
    return m


_CACHE = {}


def kernel(**inputs):
    c = Cfg()
    if "nc" not in _CACHE:
        _CACHE["nc"] = build_full(c)[1]
    nc = _CACHE["nc"]
    in_maps = [prep_inputs(c, inputs, core) for core in range(NCORE)]
    res = run_bass_kernel_spmd(nc, in_maps, core_ids=list(range(NCORE)))
    out = np.empty((c.NT, c.D), np.float32)
    for core in range(NCORE):
        o = stream_unlayout(np.asarray(res.results[core]["out"]), c.TB)
        out[core * c.TB:(core + 1) * c.TB] = o.T
    return out.reshape(c.B, c.S, c.D)
```
